# Optimizing a Trainium2 kernel written in Bass

```python
import jax
import jax.numpy as jnp
from jax import lax
import numpy as np

D_MODEL = 2048
BATCH = 2
SEQ = 8192
DEPTH = 4

GRID_W = 64
CTX_LEN = 256
N_MIXERS = 3
N_RET_LAYERS = (DEPTH + 2) // 3
N_WIN_LAYERS = (DEPTH + 1) // 3
N_FNO_LAYERS = DEPTH // 3
RET_HEADS = 8
RET_DK = D_MODEL // RET_HEADS
RET_DV = 2 * RET_DK
RET_CHUNK = 128
RET_IN_WIDTH = 2 * RET_HEADS * RET_DK + 3 * RET_HEADS * RET_DV
WIN_HEADS = 16
WIN_KV_HEADS = 4
WIN_HEAD_DIM = D_MODEL // WIN_HEADS
WINDOW = 128
WIN_BLOCK = 128
FOURIER_GROUPS = 4
N_EXPERTS = 16
EXPERT_FF = D_MODEL // 2
CAPACITY_FACTOR = 2
ROPE_BASE = 10000.0
NORM_EPS = 1e-6
NEG_INF = -1e30
F32 = jnp.float32

kernel_name = 'hybrid_retention_window_fourier_ec_moe_dit'


def rms_norm(x, gain):
    x32 = x.astype(F32)
    y = x32 * lax.rsqrt(jnp.mean(x32 * x32, axis=-1, keepdims=True) + NORM_EPS)
    return (y * gain.astype(F32)).astype(x.dtype)


def modulate(x, gain, shift, scale):
    return rms_norm(x, gain) * (1 + scale[..., None, :]) + shift[..., None, :]


def rope_1d(x, pos):
    half = x.shape[-1] // 2
    inv = ROPE_BASE ** (-jnp.arange(half, dtype=F32) / half)
    ang = pos.astype(F32)[:, None] * inv[None, :]
    cos, sin = jnp.cos(ang)[:, None, :], jnp.sin(ang)[:, None, :]
    x32 = x.astype(F32)
    x1, x2 = x32[..., :half], x32[..., half:]
    return jnp.concatenate([x1 * cos - x2 * sin, x1 * sin + x2 * cos], axis=-1).astype(x.dtype)


def axial_rope(x, row, col):
    half = x.shape[-1] // 2
    return jnp.concatenate([rope_1d(x[..., :half], row), rope_1d(x[..., half:], col)], axis=-1)


def group_norm(o):
    mu = jnp.mean(o, axis=-1, keepdims=True)
    var = jnp.mean(jnp.square(o - mu), axis=-1, keepdims=True)
    return (o - mu) * lax.rsqrt(var + NORM_EPS)


def retention_scan(q, k, v, log_gamma, state0):
    b, l, h, _ = q.shape
    dv = v.shape[-1]
    n = l // RET_CHUNK

    def chunks(t):
        return t.reshape(b, n, RET_CHUNK, h, t.shape[-1]).transpose(1, 0, 3, 2, 4)

    pos = jnp.arange(RET_CHUNK, dtype=F32)
    lg = log_gamma[:, None]
    q_decay = jnp.exp(lg * (pos + 1.0))
    k_decay = jnp.exp(lg * (RET_CHUNK - 1.0 - pos))
    chunk_decay = jnp.exp(log_gamma * RET_CHUNK)
    diff = pos[:, None] - pos[None, :]
    intra = jnp.where(diff >= 0, jnp.exp(lg[:, :, None] * jnp.maximum(diff, 0.0)), 0.0)

    def step(s, qkv):
        qc, kc, vc = qkv
        cross = jnp.einsum('bhid,bhde->bhie', qc * q_decay[None, :, :, None], s)
        scores = jnp.einsum('bhid,bhjd->bhij', qc, kc) * intra[None]
        local = jnp.einsum('bhij,bhje->bhie', scores, vc)
        s = s * chunk_decay[None, :, None, None] + jnp.einsum('bhjd,bhje->bhde', kc * k_decay[None, :, :, None], vc)
        return s, cross + local

    s_final, out = lax.scan(step, state0, (chunks(q), chunks(k), chunks(v)))
    return out.transpose(1, 0, 3, 2, 4).reshape(b, l, h, dv), s_final


def retention_mixer(h_ctx, h_lat, w_in, w_out, decay_param, row, col, with_ctx):
    b = h_lat.shape[0]
    hk, hv = RET_HEADS * RET_DK, RET_HEADS * RET_DV
    log_gamma = -jnp.exp(decay_param.astype(F32))

    def project(hh, rotate):
        l = hh.shape[1]
        q, k, v, g_f, g_b = jnp.split(hh @ w_in, [hk, 2 * hk, 2 * hk + hv, 2 * hk + 2 * hv], axis=-1)
        q = q.reshape(b, l, RET_HEADS, RET_DK)
        k = k.reshape(b, l, RET_HEADS, RET_DK) * (RET_DK ** -0.5)
        if rotate:
            q, k = axial_rope(q, row, col), axial_rope(k, row, col)
        v = v.reshape(b, l, RET_HEADS, RET_DV)
        return (q.astype(F32), k.astype(F32), v.astype(F32),
                g_f.reshape(b, l, RET_HEADS, RET_DV), g_b.reshape(b, l, RET_HEADS, RET_DV))

    qc, kc, vc, gfc, gbc = project(h_ctx, False)
    ql, kl, vl, gfl, gbl = project(h_lat, True)
    zeros = jnp.zeros((b, RET_HEADS, RET_DK, RET_DV), F32)
    flip = lambda t: jnp.flip(t, axis=1)
    oc_f, sc_f = retention_scan(qc, kc, vc, log_gamma[0], zeros)
    ol_f, _ = retention_scan(ql, kl, vl, log_gamma[0], sc_f)
    oc_b, sc_b = retention_scan(flip(qc), flip(kc), flip(vc), log_gamma[1], zeros)
    ol_b, _ = retention_scan(flip(ql), flip(kl), flip(vl), log_gamma[1], sc_b)

    def merge(o_f, o_b, g_f, g_b, dtype):
        y = (group_norm(o_f) * jax.nn.silu(g_f.astype(F32))
             + group_norm(o_b) * jax.nn.silu(g_b.astype(F32)))
        return y.reshape(y.shape[0], y.shape[1], hv).astype(dtype) @ w_out

    y_lat = merge(ol_f, flip(ol_b), gfl, gbl, h_lat.dtype)
    y_ctx = merge(oc_f, flip(oc_b), gfc, gbc, h_ctx.dtype) if with_ctx else None
    return y_ctx, y_lat


def window_attention_mixer(h_ctx, h_lat, w_qkv, w_o, sink, row, col, with_ctx):
    b, l, _ = h_lat.shape
    kv, hd = WIN_KV_HEADS, WIN_HEAD_DIM
    g = WIN_HEADS // kv
    scale = hd ** -0.5

    def project(hh):
        n = hh.shape[1]
        q, k, v = jnp.split(hh @ w_qkv, [WIN_HEADS * hd, (WIN_HEADS + kv) * hd], axis=-1)
        return q.reshape(b, n, WIN_HEADS, hd), k.reshape(b, n, kv, hd), v.reshape(b, n, kv, hd)

    qc, kc, vc = project(h_ctx)
    ql, kl, vl = project(h_lat)
    ql, kl = axial_rope(ql, row, col), axial_rope(kl, row, col)
    sink_logit = sink.astype(F32).reshape(kv, g)

    def softmax_with_sink(logits):
        sk = jnp.broadcast_to(sink_logit[:, :, None, None], logits.shape[:-1] + (1,))
        return jax.nn.softmax(jnp.concatenate([logits, sk], axis=-1), axis=-1)[..., :-1]

    nb = l // WIN_BLOCK
    pad = ((0, 0), (WIN_BLOCK, WIN_BLOCK), (0, 0), (0, 0))
    kb = jnp.pad(kl, pad).reshape(b, nb + 2, WIN_BLOCK, kv, hd)
    vb = jnp.pad(vl, pad).reshape(b, nb + 2, WIN_BLOCK, kv, hd)
    band = lambda t: jnp.concatenate([t[:, :-2], t[:, 1:-1], t[:, 2:]], axis=2).transpose(1, 0, 2, 3, 4)
    k_band, v_band = band(kb), band(vb)
    q_blocks = ql.reshape(b, nb, WIN_BLOCK, kv, g, hd).transpose(1, 0, 2, 3, 4, 5)
    qpos = jnp.arange(nb)[:, None] * WIN_BLOCK + jnp.arange(WIN_BLOCK)[None, :]
    kpos = (jnp.arange(nb)[:, None] - 1) * WIN_BLOCK + jnp.arange(3 * WIN_BLOCK)[None, :]
    kp = kpos[:, None, :]
    valid = (jnp.abs(kp - qpos[:, :, None]) <= WINDOW) & (kp >= 0) & (kp < l)
    bias = jnp.where(valid, 0.0, NEG_INF).astype(F32)
    n_ctx = kc.shape[1]

    def block(args):
        qb, kbd, vbd, bb = args
        s_ctx = jnp.einsum('bqkgd,bckd->bkgqc', qb, kc).astype(F32) * scale
        s_loc = jnp.einsum('bqkgd,bskd->bkgqs', qb, kbd).astype(F32) * scale + bb
        p = softmax_with_sink(jnp.concatenate([s_ctx, s_loc], axis=-1))
        p_ctx, p_loc = p[..., :n_ctx].astype(vc.dtype), p[..., n_ctx:].astype(vbd.dtype)
        return jnp.einsum('bkgqc,bckd->bqkgd', p_ctx, vc) + jnp.einsum('bkgqs,bskd->bqkgd', p_loc, vbd)

    o = lax.map(block, (q_blocks, k_band, v_band, bias))
    y_lat = o.transpose(1, 0, 2, 3, 4, 5).reshape(b, l, WIN_HEADS * hd) @ w_o
    y_ctx = None
    if with_ctx:
        qcg = qc.reshape(b, n_ctx, kv, g, hd)
        p = softmax_with_sink(jnp.einsum('bqkgd,bckd->bkgqc', qcg, kc).astype(F32) * scale).astype(vc.dtype)
        y_ctx = jnp.einsum('bkgqc,bckd->bqkgd', p, vc).reshape(b, n_ctx, WIN_HEADS * hd) @ w_o
    return y_ctx, y_lat


def fourier_mixer(h_ctx, h_lat, w_o, with_ctx):
    def mix(hh):
        b, n, d = hh.shape
        z = hh.astype(F32).reshape(b, n, FOURIER_GROUPS, d // FOURIER_GROUPS)
        z = jnp.fft.fftn(z, axes=(1, 3), norm='ortho').real
        return z.reshape(b, n, d).astype(hh.dtype) @ w_o

    return (mix(h_ctx) if with_ctx else None), mix(h_lat)


def ec_moe(h, w_router, w_gate, w_up, w_down):
    b, n, d = h.shape
    cap = CAPACITY_FACTOR * n // N_EXPERTS
    aff = jax.nn.softmax((h @ w_router).astype(F32), axis=-1)
    gate, idx = lax.top_k(jnp.swapaxes(aff, 1, 2), cap)
    xe = jax.vmap(lambda hb, ib: hb[ib])(h, idx)
    a = jnp.einsum('becd,edf->becf', xe, w_gate)
    u = jnp.einsum('becd,edf->becf', xe, w_up)
    ye = jnp.einsum('becf,efd->becd', jax.nn.silu(a) * u, w_down) * gate[..., None].astype(h.dtype)
    return jax.vmap(lambda yb, ib: jnp.zeros((n, d), yb.dtype).at[ib.reshape(-1)].add(yb.reshape(-1, d)))(ye, idx)


def setup_inputs(seed: int = 0) -> dict:
    key = jax.random.key(seed)
    ks = jax.random.split(key, 19)
    nrm = lambda k, shape, s: jax.random.normal(k, shape, F32) * s
    d, f = D_MODEL, EXPERT_FF
    gamma = 1.0 - jnp.power(2.0, -(5.0 + jnp.arange(RET_HEADS, dtype=F32)))
    decay_base = jnp.log(-jnp.log(gamma))
    return {
        'x': nrm(ks[0], (BATCH, SEQ, d), 1.0),
        'c': nrm(ks[1], (BATCH, d), 1.0),
        'ctx': nrm(ks[2], (BATCH, CTX_LEN, d), 1.0),
        'c_ctx': nrm(ks[3], (d,), 1.0),
        'w_mod': nrm(ks[4], (DEPTH, d, 6 * d), 0.5 * d ** -0.5),
        'b_mod': nrm(ks[5], (DEPTH, 6 * d), 0.02),
        'norm_gain': 1.0 + nrm(ks[6], (DEPTH, 2, d), 0.02),
        'final_gain': 1.0 + nrm(ks[7], (d,), 0.02),
        'ret_w_in': nrm(ks[8], (N_RET_LAYERS, d, RET_IN_WIDTH), d ** -0.5),
        'ret_w_out': nrm(ks[9], (N_RET_LAYERS, RET_HEADS * RET_DV, d), (RET_HEADS * RET_DV) ** -0.5),
        'ret_decay': decay_base[None, None, :] + nrm(ks[10], (N_RET_LAYERS, 2, RET_HEADS), 0.1),
        'win_w_qkv': nrm(ks[11], (N_WIN_LAYERS, d, (WIN_HEADS + 2 * WIN_KV_HEADS) * WIN_HEAD_DIM), d ** -0.5),
        'win_w_o': nrm(ks[12], (N_WIN_LAYERS, WIN_HEADS * WIN_HEAD_DIM, d), (WIN_HEADS * WIN_HEAD_DIM) ** -0.5),
        'win_sink': nrm(ks[13], (N_WIN_LAYERS, WIN_HEADS), 1.0),
        'fno_w_o': nrm(ks[14], (N_FNO_LAYERS, d, d), d ** -0.5),
        'router_w': nrm(ks[15], (DEPTH, d, N_EXPERTS), d ** -0.5),
        'exp_w_gate': nrm(ks[16], (DEPTH, N_EXPERTS, d, f), d ** -0.5),
        'exp_w_up': nrm(ks[17], (DEPTH, N_EXPERTS, d, f), d ** -0.5),
        'exp_w_down': nrm(ks[18], (DEPTH, N_EXPERTS, f, d), f ** -0.5),
    }


def reference(x, c, ctx, c_ctx, w_mod, b_mod, norm_gain, final_gain, ret_w_in, ret_w_out, ret_decay,
              win_w_qkv, win_w_o, win_sink, fno_w_o, router_w, exp_w_gate, exp_w_up, exp_w_down):
    l = x.shape[1]
    rows = l // GRID_W
    row = jnp.repeat(jnp.arange(rows), GRID_W)
    col = jnp.tile(jnp.arange(GRID_W), rows)
    silu_c, silu_cc = jax.nn.silu(c), jax.nn.silu(c_ctx)
    for i in range(DEPTH):
        kind, j = i % N_MIXERS, i // N_MIXERS
        last = i == DEPTH - 1
        sh1, sc1, g1, sh2, sc2, g2 = jnp.split(silu_c @ w_mod[i] + b_mod[i], 6, axis=-1)
        csh1, csc1, cg1, csh2, csc2, cg2 = jnp.split(silu_cc @ w_mod[i] + b_mod[i], 6, axis=-1)
        h_lat = modulate(x, norm_gain[i, 0], sh1, sc1)
        h_ctx = modulate(ctx, norm_gain[i, 0], csh1, csc1)
        if kind == 0:
            y_ctx, y_lat = retention_mixer(h_ctx, h_lat, ret_w_in[j], ret_w_out[j], ret_decay[j], row, col, not last)
        elif kind == 1:
            y_ctx, y_lat = window_attention_mixer(h_ctx, h_lat, win_w_qkv[j], win_w_o[j], win_sink[j], row, col, not last)
        else:
            y_ctx, y_lat = fourier_mixer(h_ctx, h_lat, fno_w_o[j], not last)
        x = x + g1[:, None, :] * y_lat
        h2 = modulate(x, norm_gain[i, 1], sh2, sc2)
        x = x + g2[:, None, :] * ec_moe(h2, router_w[i], exp_w_gate[i], exp_w_up[i], exp_w_down[i])
        if not last:
            ctx = ctx + cg1 * y_ctx
            h2c = modulate(ctx, norm_gain[i, 1], csh2, csc2)
            ctx = ctx + cg2 * ec_moe(h2c, router_w[i], exp_w_gate[i], exp_w_up[i], exp_w_down[i])
    return rms_norm(x, final_gain)
```

```python
import numpy as np
from contextlib import ExitStack
import concourse.bass as bass
import concourse.mybir as mybir
from concourse.bass_utils import run_bass_kernel_spmd

F32 = mybir.dt.float32
BF16 = mybir.dt.bfloat16
I32 = mybir.dt.int32
AF = mybir.ActivationFunctionType
ALU = mybir.AluOpType
AX = mybir.AxisListType

N_CORES = 8


class Dep:
    __slots__ = ("w", "r")

    def __init__(self, name=""):
        self.w = {}
        self.r = {}


class Prog:
    EPOCH = 30000
    NDMA = 12

    def __init__(self, nc):
        self.nc = nc
        self.es = ExitStack()
        self.root = self.es
        self.eng = {"pe": nc.tensor, "act": nc.scalar, "dve": nc.vector, "pool": nc.gpsimd, "sp": nc.sync}
        self.sem = {}
        self.cnt = {}
        self.nsem = 0
        for k in ("pe", "act", "dve", "pool"):
            self._new_sem(k)
        self.waited = {}
        self.dq = {}
        for q in ("sp", "act", "pool"):
            sems = [self.es.enter_context(nc.semaphore("d_%s%d" % (q, i))) for i in range(self.NDMA)]
            self.dq[q] = {"sems": sems, "uses": [0] * self.NDMA, "next": 0}
        self.n_inst = 0
        self.bc_reg = {}

    def bounds_reg(self, val):
        if val not in self.bc_reg:
            r = self.root.enter_context(self.nc.gpsimd.register("bc%d" % val))
            self.nc.gpsimd.reg_mov(r, val)
            self.bc_reg[val] = r
        return self.bc_reg[val]

    def _new_sem(self, k):
        self.nsem += 1
        self.sem[k] = self.root.enter_context(self.nc.semaphore("s_%s_%d" % (k, self.nsem)))
        self.cnt[k] = 0

    def sb(self, name, shape, dtype):
        self.n_names = getattr(self, "n_names", 0) + 1
        return self.es.enter_context(self.nc.sbuf_tensor("%s_%d" % (name, self.n_names), list(shape), dtype))

    def ps(self, name, shape, dtype=F32):
        self.n_names = getattr(self, "n_names", 0) + 1
        esz = 4 if dtype == F32 else 2
        nel = 1
        for d_ in shape[1:]:
            nel *= d_
        per_bank = 2048 // esz
        padded = ((nel + per_bank - 1) // per_bank) * per_bank
        t = self.es.enter_context(self.nc.psum_tensor("%s_%d" % (name, self.n_names), [shape[0], padded], dtype))
        v = t[:, 0:nel]
        if len(shape) == 3:
            v = v.rearrange("p (a b) -> p a b", a=shape[1])
        return v

    def _wait(self, engname, ev):
        if ev is None:
            return
        sem, val = ev[0], ev[1]
        if val <= 0:
            return
        key = (engname, id(sem))
        if self.waited.get(key, 0) >= val:
            return
        self.eng[engname].wait_ge(sem, val)
        self.waited[key] = val

    def _deps(self, engname, reads, writes):
        for d in reads:
            for ev in d.w.values():
                if not (engname == "pe" and ev[2] == "pe"):
                    self._wait(engname, ev)
        for d in writes:
            for ev in d.w.values():
                if not (engname == "pe" and ev[2] == "pe"):
                    self._wait(engname, ev)
            for ev in d.r.values():
                if not (engname == "pe" and ev[2] == "pe"):
                    self._wait(engname, ev)

    def _commit(self, engname, ev, reads, writes, wshared=()):
        ev3 = (ev[0], ev[1], engname)
        k = id(ev[0])
        for d in reads:
            d.r[k] = ev3
        for d in writes:
            d.w = {k: ev3}
            d.r = {}
        for d in wshared:
            d.w[k] = ev3

    def op(self, engname, fn, reads=(), writes=(), signal=True):
        self._deps(engname, reads, writes)
        ins = fn(self.eng[engname])
        self.n_inst += 1
        if self.cnt[engname] >= self.EPOCH:
            self._new_sem(engname)
        if signal:
            self.cnt[engname] += 1
            ins.then_inc(self.sem[engname], 1)
            ev = (self.sem[engname], self.cnt[engname])
        else:
            ev = (self.sem[engname], self.cnt[engname] + 1)
        self._commit(engname, ev, reads, writes)
        return ins

    def dma(self, q, out, in_, reads=(), writes=(), wshared=(), indirect=None, **kw):
        pool = self.dq[q]
        k = pool["next"]
        pool["next"] = (k + 1) % self.NDMA
        sem = pool["sems"][k]
        uses = pool["uses"][k]
        self._wait(q, (sem, 16 * uses))
        self._deps(q, reads, writes)
        e = self.eng[q]
        if indirect is None:
            ins = e.dma_start(out=out, in_=in_, **kw)
        else:
            ins = e.indirect_dma_start(out=out, in_=in_, **indirect)
        ins.then_inc(sem, 16)
        self.n_inst += 1
        pool["uses"][k] = uses + 1
        ev = (sem, 16 * (uses + 1))
        self._commit(q + "_dma", ev, reads, writes, wshared)
        return ins

    def finish(self, deps):
        for d in deps:
            for ev in d.w.values():
                self._wait("sp", ev)

    def close(self):
        self.es.close()


class _Scope:
    def __init__(self, P):
        self.P = P

    def __enter__(self):
        self.saved = self.P.es
        self.P.es = ExitStack()
        return self

    def __exit__(self, *a):
        self.P.barrier()
        self.P.es.close()
        self.P.es = self.saved
        return False


def _scope(self):
    return _Scope(self)


def _barrier(self):
    evs = []
    for k in ("pe", "act", "dve", "pool"):
        if self.cnt[k] > 0:
            evs.append((self.sem[k], self.cnt[k]))
    for q in ("sp", "act", "pool"):
        pool = self.dq[q]
        for s, u in zip(pool["sems"], pool["uses"]):
            if u > 0:
                evs.append((s, 16 * u))
    for e in ("pe", "act", "dve", "pool", "sp"):
        for ev in evs:
            self._wait(e, ev)


Prog.scope = _scope
Prog.barrier = _barrier

D = 2048
NT = 66
T = NT * 128
NCTX_T = 2
EPS = 1e-6


def copy_evac(P, i, out, in_, reads, writes):
    if i % 2 == 0:
        P.op("act", lambda e: e.copy(out=out, in_=in_), reads=reads, writes=writes)
    else:
        P.op("dve", lambda e: e.tensor_copy(out=out, in_=in_), reads=reads, writes=writes)


def make_ident(P):
    idf = P.sb("idf", [128, 128], F32)
    ident = P.sb("ident", [128, 128], BF16)
    d1, d2 = Dep(), Dep()
    P.op("pool", lambda e: e.iota(idf[:], pattern=[[1, 128]], base=0, channel_multiplier=-1,
                                  allow_small_or_imprecise_dtypes=True), writes=[d1])
    P.op("dve", lambda e: e.tensor_single_scalar(out=ident[:], in_=idf[:], scalar=0.0, op=ALU.is_equal),
         reads=[d1], writes=[d2])
    return ident, d2


def project(P, src, K, W, N, dst, dst_dtype, tiles, tag):
    KC = K // 128
    ST = 16 if KC <= 16 else 8
    CB = min(N, 512)
    ncb = N // CB
    Wv = W.rearrange("(j p) n -> p j n", p=128)
    with P.scope():
        ident, d_id = make_ident(P)
        srcT = P.sb(tag + "srcT", [128, KC, ST * 128], BF16)
        d_srcT = [Dep() for _ in range(ST)]
        hin = [P.sb(tag + "hin%d" % i, [128, K], BF16) for i in range(2)]
        d_hin = [Dep(), Dep()]
        wblk = [P.sb(tag + "w%d" % i, [128, KC, CB], BF16) for i in range(2)]
        d_w = [Dep(), Dep()]
        pT = P.ps(tag + "pT", [128, 16, 128], BF16)
        d_pT = Dep()
        pO = [P.ps(tag + "pO%d" % i, [128, 512], F32) for i in range(4)]
        d_pO = [Dep() for _ in range(4)]
        stg = [P.sb(tag + "stg%d" % i, [128, CB], dst_dtype) for i in range(4)]
        d_stg = [Dep() for _ in range(4)]
        n = 0
        nl = 0
        nw = 0
        for s0 in range(0, len(tiles), ST):
            sts = tiles[s0:s0 + ST]
            for ti, t in enumerate(sts):
                b = nl % 2
                nl += 1
                P.dma("sp", hin[b][:], src[t * 128:(t + 1) * 128, :], writes=[d_hin[b]])
                for k0 in range(0, KC, 16):
                    for kc in range(k0, k0 + 16):
                        P.op("pe", lambda e: e.transpose(pT[:, kc - k0, :], hin[b][:, kc * 128:(kc + 1) * 128], ident[:]),
                             reads=[d_hin[b], d_id], writes=[d_pT], signal=(kc == k0 + 15))
                    copy_evac(P, nl + k0 // 16, srcT[:, k0:k0 + 16, ti * 128:(ti + 1) * 128], pT[:], [d_pT], [d_srcT[ti]])
            for cb in range(ncb):
                wb = nw % 2
                nw += 1
                for k0 in range(0, KC, 16):
                    P.dma("pool", wblk[wb][:, k0:k0 + 16, :], Wv[:, k0:k0 + 16, cb * CB:(cb + 1) * CB],
                          writes=[d_w[wb]] if k0 == 0 else [], wshared=[d_w[wb]] if k0 else [])
                for ti, t in enumerate(sts):
                    o = n % 4
                    n += 1
                    for kc in range(KC):
                        P.op("pe", lambda e: e.matmul(pO[o][:, :CB], lhsT=srcT[:, kc, ti * 128:(ti + 1) * 128],
                                                      rhs=wblk[wb][:, kc, :], start=(kc == 0), stop=(kc == KC - 1)),
                             reads=[d_srcT[ti], d_w[wb]], writes=[d_pO[o]], signal=(kc == KC - 1))
                    copy_evac(P, n, stg[o][:], pO[o][:, :CB], [d_pO[o]], [d_stg[o]])
                    P.dma("sp", dst(t, cb * CB, CB), stg[o][:], reads=[d_stg[o]])


def load_bc(P, q, out, vec_ap, dep_out):
    P.dma(q, out, vec_ap.partition_broadcast(128), writes=[dep_out])


def mod_phase(P, cvec, w_mod, b_mod, MOD):
    with P.scope():
        cv = P.sb("m_cv", [128, 2, 16], F32)
        d_cv = Dep()
        for s_ in range(2):
            P.dma("sp", cv[:, s_, :], cvec[s_].rearrange("(p j) -> p j", j=16), wshared=[d_cv])
        sc = P.sb("m_sc", [128, 2, 16], F32)
        d_sc = Dep()
        P.op("act", lambda e: e.activation(out=sc[:], in_=cv[:], func=AF.Silu), reads=[d_cv], writes=[d_sc])
        lhsT = P.sb("m_lhsT", [128, 16, 128], BF16)
        d_l = Dep()
        for s_ in range(2):
            P.op("dve", lambda e: e.tensor_copy(out=lhsT[:, :, s_ * 64:(s_ + 1) * 64],
                                                in_=sc[:, s_, :].unsqueeze(2).to_broadcast([128, 16, 64])),
                 reads=[d_sc], writes=[d_l])
        wb = [P.sb("m_w%d" % i, [128, 16, 512], BF16) for i in range(2)]
        d_w = [Dep(), Dep()]
        bb = [P.sb("m_b%d" % i, [128, 512], F32) for i in range(2)]
        d_b = [Dep(), Dep()]
        po = [P.ps("m_po%d" % i, [128, 512], F32) for i in range(2)]
        d_po = [Dep(), Dep()]
        st = [P.sb("m_st%d" % i, [128, 512], F32) for i in range(2)]
        d_st = [Dep(), Dep()]
        n = 0
        for l in range(4):
            Wv = w_mod[l].rearrange("(p j) n -> p j n", j=16)
            for cb in range(24):
                i = n % 2
                n += 1
                cs = slice(cb * 512, (cb + 1) * 512)
                P.dma("pool", wb[i][:], Wv[:, :, cs], writes=[d_w[i]])
                load_bc(P, "sp", bb[i][:], b_mod[l, cs], d_b[i])
                for j in range(16):
                    P.op("pe", lambda e: e.matmul(po[i][:], lhsT=lhsT[:, j, :], rhs=wb[i][:, j, :], start=(j == 0), stop=(j == 15)),
                         reads=[d_l, d_w[i]], writes=[d_po[i]], signal=(j == 15))
                P.op("dve", lambda e: e.tensor_add(out=st[i][:], in0=po[i][:], in1=bb[i][:]), reads=[d_po[i], d_b[i]], writes=[d_st[i]])
                P.dma("sp", MOD[l, 0:1, cs], st[i][0:1, :], reads=[d_st[i]])
                P.dma("sp", MOD[l, 1:2, cs], st[i][64:65, :], reads=[d_st[i]])


def norm_phase(P, X, MOD, layer, which, gain, H, resid=None, final_out=None, Xsrc=None):
    with P.scope():
        A = [P.sb("n_A%d" % s_, [128, D], F32) for s_ in range(2)]
        SH = [P.sb("n_SH%d" % s_, [128, D], F32) for s_ in range(2)]
        d_A = [Dep(), Dep()]
        d_SH = [Dep(), Dep()]
        gn = P.sb("n_gain", [128, D], F32)
        d_gn = Dep()
        load_bc(P, "sp", gn[:], gain, d_gn)
        if final_out is None:
            for s_ in range(2):
                load_bc(P, "sp", A[s_][:], MOD[layer, s_, (3 * which + 1) * D:(3 * which + 2) * D], d_A[s_])
                load_bc(P, "sp", SH[s_][:], MOD[layer, s_, (3 * which) * D:(3 * which + 1) * D], d_SH[s_])
                P.op("dve", lambda e: e.scalar_tensor_tensor(out=A[s_][:], in0=A[s_][:], scalar=1.0, in1=gn[:], op0=ALU.add, op1=ALU.mult),
                     reads=[d_A[s_], d_gn], writes=[d_A[s_]])
        if resid is not None:
            G = [P.sb("n_G%d" % s_, [128, D], F32) for s_ in range(2)]
            d_G = [Dep(), Dep()]
            for s_ in range(2):
                load_bc(P, "sp", G[s_][:], MOD[layer, s_, resid[1] * D:(resid[1] + 1) * D], d_G[s_])
            yt = [P.sb("n_y%d" % i, [128, D], F32) for i in range(2)]
            d_y = [Dep(), Dep()]
        xt = [P.sb("n_x%d" % i, [128, D], F32) for i in range(2)]
        d_x = [Dep(), Dep()]
        tmp = [P.sb("n_t%d" % i, [128, D], F32) for i in range(2)]
        d_t = [Dep(), Dep()]
        ht = [P.sb("n_h%d" % i, [128, D], BF16 if final_out is None else F32) for i in range(2)]
        d_h = [Dep(), Dep()]
        ss = [P.sb("n_ss%d" % i, [128, 1], F32) for i in range(2)]
        d_ss = [Dep(), Dep()]
        tiles = range(NT) if final_out is None else range(NCTX_T, NT)
        for n, t in enumerate(tiles):
            i = n % 2
            s_ = 1 if t < NCTX_T else 0
            rows = slice(t * 128, (t + 1) * 128)
            P.dma("sp", xt[i][:], (X if Xsrc is None else Xsrc)[rows, :], writes=[d_x[i]])
            if resid is not None:
                P.dma("sp", yt[i][:], resid[0][rows, :], writes=[d_y[i]])
                P.op("pool", lambda e: e.tensor_mul(out=yt[i][:], in0=yt[i][:], in1=G[s_][:]), reads=[d_y[i], d_G[s_]], writes=[d_y[i]])
                P.op("dve", lambda e: e.tensor_add(out=xt[i][:], in0=xt[i][:], in1=yt[i][:]), reads=[d_y[i], d_x[i]], writes=[d_x[i]])
                P.dma("pool", X[rows, :], xt[i][:], reads=[d_x[i]])
            P.op("act", lambda e: e.activation(out=tmp[i][:], in_=xt[i][:], func=AF.Square, accum_out=ss[i][:]),
                 reads=[d_x[i]], writes=[d_t[i], d_ss[i]])
            P.op("act", lambda e: e.activation(out=ss[i][:], in_=ss[i][:], func=AF.Sqrt, bias=EPS, scale=1.0 / D),
                 reads=[d_ss[i]], writes=[d_ss[i]])
            P.op("dve", lambda e: e.reciprocal(out=ss[i][:], in_=ss[i][:]), reads=[d_ss[i]], writes=[d_ss[i]])
            if final_out is None:
                P.op("dve", lambda e: e.scalar_tensor_tensor(out=tmp[i][:], in0=xt[i][:], scalar=ss[i][:, 0:1], in1=A[s_][:],
                                                             op0=ALU.mult, op1=ALU.mult),
                     reads=[d_x[i], d_ss[i], d_A[s_]], writes=[d_t[i]])
                P.op("pool", lambda e: e.tensor_add(out=ht[i][:], in0=tmp[i][:], in1=SH[s_][:]), reads=[d_t[i], d_SH[s_]], writes=[d_h[i]])
                P.dma("pool", H[rows, :], ht[i][:], reads=[d_h[i]])
            else:
                P.op("dve", lambda e: e.scalar_tensor_tensor(out=ht[i][:], in0=xt[i][:], scalar=ss[i][:, 0:1], in1=gn[:],
                                                             op0=ALU.mult, op1=ALU.mult),
                     reads=[d_x[i], d_ss[i], d_gn], writes=[d_h[i]])
                P.dma("pool", final_out[(t - NCTX_T) * 128:(t - NCTX_T + 1) * 128, :], ht[i][:], reads=[d_h[i]])


NE = 16
CAP_LAT = 1024
CAP_CTX = 32
HW = D + 48
BIG = 4096.0
NBIS = 34


def routing_phase(P, LOGITS, consts, R):
    aff, idx = R["aff"], R["idx"]
    d_aff, d_idx = R["d_aff"], R["d_idx"]
    with P.scope():
        lg = P.sb("r_lg", [128, NT, NE], F32)
        d_lg = Dep()
        Lv = LOGITS.rearrange("(n p) e -> p n e", p=128)
        for n in range(NT):
            P.dma("sp", lg[:, n, :], Lv[:, n, :], wshared=[d_lg])
        mx = P.sb("r_mx", [128, NT], F32)
        d_mx = Dep()
        P.op("dve", lambda e: e.tensor_reduce(out=mx[:], in_=lg[:], axis=AX.X, op=ALU.max), reads=[d_lg], writes=[d_mx])
        P.op("dve", lambda e: e.tensor_tensor(out=lg[:], in0=lg[:], in1=mx[:].unsqueeze(2).to_broadcast([128, NT, NE]), op=ALU.subtract),
             reads=[d_lg, d_mx], writes=[d_lg])
        P.op("act", lambda e: e.activation(out=lg[:], in_=lg[:], func=AF.Exp), reads=[d_lg], writes=[d_lg])
        P.op("dve", lambda e: e.tensor_reduce(out=mx[:], in_=lg[:], axis=AX.X, op=ALU.add), reads=[d_lg], writes=[d_mx])
        P.op("dve", lambda e: e.reciprocal(out=mx[:], in_=mx[:]), reads=[d_mx], writes=[d_mx])
        P.op("dve", lambda e: e.tensor_tensor(out=aff[:], in0=lg[:], in1=mx[:].unsqueeze(2).to_broadcast([128, NT, NE]), op=ALU.mult),
             reads=[d_lg, d_mx], writes=[d_aff])
        aff3 = R["aff3"]
        d_aff3 = R["d_aff3"]
        rsd = P.sb("r_rsd", [128, NT, NE], F32)
        d_rsd = Dep()
        P.op("dve", lambda e: e.tensor_copy(out=aff3[:, :, :, 0], in_=aff[:]), reads=[d_aff], writes=[d_aff3])
        P.op("dve", lambda e: e.tensor_tensor(out=rsd[:], in0=aff[:], in1=aff3[:, :, :, 0], op=ALU.subtract), reads=[d_aff, d_aff3], writes=[d_rsd])
        P.op("dve", lambda e: e.tensor_copy(out=aff3[:, :, :, 1], in_=rsd[:]), reads=[d_rsd], writes=[d_aff3])
        P.op("dve", lambda e: e.tensor_tensor(out=rsd[:], in0=rsd[:], in1=aff3[:, :, :, 1], op=ALU.subtract), reads=[d_rsd, d_aff3], writes=[d_rsd])
        P.op("dve", lambda e: e.tensor_copy(out=aff3[:, :, :, 2], in_=rsd[:]), reads=[d_rsd], writes=[d_aff3])
        ones = P.sb("r_ones", [128, 128], BF16)
        d_ones = Dep()
        P.op("pool", lambda e: e.memset(ones[:], 1.0), writes=[d_ones])
        tri = P.sb("r_tri", [128, 128], BF16)
        trif = P.sb("r_trif", [128, 128], F32)
        d_tri = Dep()
        P.op("pool", lambda e: e.iota(trif[:], pattern=[[1, 128]], base=0, channel_multiplier=-1,
                                      allow_small_or_imprecise_dtypes=True), writes=[d_tri])
        P.op("dve", lambda e: e.tensor_single_scalar(out=tri[:], in_=trif[:], scalar=0.0, op=ALU.is_gt), reads=[d_tri], writes=[d_tri])
        kv = P.sb("r_kv", [128, 2, NE], F32)
        d_kv = Dep()
        P.op("pool", lambda e: e.memset(kv[:, 0, :], float(CAP_CTX)), wshared=[]) if False else None
        P.op("pool", lambda e: e.memset(kv[:, 0, :], float(CAP_CTX)), writes=[d_kv])
        P.op("pool", lambda e: e.memset(kv[:, 1, :], float(CAP_LAT)), writes=[d_kv])
        lo = P.sb("r_lo", [128, 2, NE], F32)
        hi = P.sb("r_hi", [128, 2, NE], F32)
        mid = P.sb("r_mid", [128, 2, NE], F32)
        gt = P.sb("r_gt", [128, 2, NE], F32)
        t1 = P.sb("r_t1", [128, 2, NE], F32)
        cntp = P.sb("r_cntp", [128, 2, NE], BF16)
        cntf = P.sb("r_cntf", [128, 2, NE], F32)
        d_cntf = Dep()
        d_lo, d_hi, d_mid, d_gt, d_t1, d_cntp = Dep(), Dep(), Dep(), Dep(), Dep(), Dep()
        P.op("pool", lambda e: e.memset(lo[:], 0.0), writes=[d_lo])
        P.op("pool", lambda e: e.memset(hi[:], 1.0), writes=[d_hi])
        cmp_ = P.sb("r_cmp", [128, NT, NE], F32)
        d_cmp = Dep()
        pc = P.ps("r_pc", [128, 2 * NE], F32)
        d_pc = Dep()
        segs = [(0, 0, NCTX_T), (1, NCTX_T, NT)]

        def count_gt(thr, d_thr):
            for s_, a, b in segs:
                P.op("dve", lambda e: e.tensor_tensor(out=cmp_[:, a:b, :], in0=aff[:, a:b, :],
                                                      in1=thr[:, s_:s_ + 1, :].to_broadcast([128, b - a, NE]), op=ALU.is_gt),
                     reads=[d_aff, d_thr], writes=[d_cmp])
                P.op("dve", lambda e: e.tensor_reduce(out=cntf[:, s_, :], in_=cmp_[:, a:b, :].rearrange("p n e -> p e n"),
                                                      axis=AX.X, op=ALU.add), reads=[d_cmp], writes=[d_cntf])
            P.op("dve", lambda e: e.tensor_copy(out=cntp[:], in_=cntf[:]), reads=[d_cntf], writes=[d_cntp])
            P.op("pe", lambda e: e.matmul(pc[:], lhsT=ones[:], rhs=cntp[:].rearrange("p s e -> p (s e)"), start=True, stop=True),
                 reads=[d_ones, d_cntp], writes=[d_pc])

        for it in range(NBIS):
            P.op("dve", lambda e: e.tensor_tensor(out=mid[:], in0=lo[:], in1=hi[:], op=ALU.add), reads=[d_lo, d_hi], writes=[d_mid])
            P.op("dve", lambda e: e.tensor_single_scalar(out=mid[:], in_=mid[:], scalar=0.5, op=ALU.mult), reads=[d_mid], writes=[d_mid])
            count_gt(mid, d_mid)
            P.op("dve", lambda e: e.tensor_tensor(out=gt[:].rearrange("p s e -> p (s e)"), in0=pc[:],
                                                  in1=kv[:].rearrange("p s e -> p (s e)"), op=ALU.is_gt),
                 reads=[d_pc, d_kv], writes=[d_gt])
            P.op("dve", lambda e: e.tensor_tensor(out=t1[:], in0=mid[:], in1=lo[:], op=ALU.subtract), reads=[d_mid, d_lo], writes=[d_t1])
            P.op("dve", lambda e: e.tensor_tensor(out=t1[:], in0=t1[:], in1=gt[:], op=ALU.mult), reads=[d_t1, d_gt], writes=[d_t1])
            P.op("dve", lambda e: e.tensor_tensor(out=lo[:], in0=lo[:], in1=t1[:], op=ALU.add), reads=[d_t1, d_lo], writes=[d_lo])
            P.op("dve", lambda e: e.tensor_tensor(out=t1[:], in0=hi[:], in1=mid[:], op=ALU.subtract), reads=[d_mid, d_hi], writes=[d_t1])
            P.op("dve", lambda e: e.tensor_tensor(out=t1[:], in0=t1[:], in1=gt[:], op=ALU.mult), reads=[d_t1, d_gt], writes=[d_t1])
            P.op("dve", lambda e: e.tensor_tensor(out=hi[:], in0=mid[:], in1=t1[:], op=ALU.add), reads=[d_t1, d_mid], writes=[d_hi])
        msk = P.sb("r_msk", [128, NT, NE], BF16)
        d_msk = Dep()
        for s_, a, b in segs:
            P.op("dve", lambda e: e.tensor_tensor(out=msk[:, a:b, :], in0=aff[:, a:b, :],
                                                  in1=hi[:, s_:s_ + 1, :].to_broadcast([128, b - a, NE]), op=ALU.is_gt),
                 reads=[d_aff, d_hi], writes=[d_msk])
        P.op("dve", lambda e: e.tensor_copy(out=R["sel"][:], in_=msk[:]), reads=[d_msk], writes=[R["d_sel"]])
        mflat = msk[:].rearrange("p n e -> p (n e)")
        W_ = NT * NE
        pp = [P.ps("r_pp%d" % i, [128, 512], F32) for i in range(3)]
        pt = [P.ps("r_pt%d" % i, [128, 512], F32) for i in range(3)]
        d_pp, d_pt = Dep(), Dep()
        slot = P.sb("r_slot", [128, NT, NE], F32)
        tot = P.sb("r_tot", [128, NT, NE], F32)
        d_slot, d_tot = Dep(), Dep()
        sflat = slot[:].rearrange("p n e -> p (n e)")
        tflat = tot[:].rearrange("p n e -> p (n e)")
        for i in range(3):
            c0, c1 = i * 512, min(W_, (i + 1) * 512)
            P.op("pe", lambda e: e.matmul(pp[i][:, :c1 - c0], lhsT=tri[:], rhs=mflat[:, c0:c1], start=True, stop=True),
                 reads=[d_tri, d_msk], writes=[d_pp])
            P.op("pe", lambda e: e.matmul(pt[i][:, :c1 - c0], lhsT=ones[:], rhs=mflat[:, c0:c1], start=True, stop=True),
                 reads=[d_ones, d_msk], writes=[d_pt])
            P.op("act", lambda e: e.copy(out=sflat[:, c0:c1], in_=pp[i][:, :c1 - c0]), reads=[d_pp], writes=[d_slot])
            P.op("act", lambda e: e.copy(out=tflat[:, c0:c1], in_=pt[i][:, :c1 - c0]), reads=[d_pt], writes=[d_tot])
        off = P.sb("r_off", [128, NE], F32)
        d_off = Dep()
        for s_, a, b in segs:
            P.op("dve", lambda e: e.memset(off[:], 0.0), writes=[d_off])
            for n in range(a, b):
                P.op("dve", lambda e: e.tensor_tensor(out=slot[:, n, :], in0=slot[:, n, :], in1=off[:], op=ALU.add),
                     reads=[d_slot, d_off, d_tot], writes=[d_slot])
                if n < b - 1:
                    P.op("dve", lambda e: e.tensor_tensor(out=off[:], in0=off[:], in1=tot[:, n, :], op=ALU.add),
                         reads=[d_tot, d_off, d_slot], writes=[d_off])
        P.op("dve", lambda e: e.scalar_tensor_tensor(out=slot[:], in0=msk[:], scalar=-BIG, in1=slot[:], op0=ALU.mult, op1=ALU.add),
             reads=[d_msk, d_slot], writes=[d_slot])
        P.op("dve", lambda e: e.tensor_single_scalar(out=slot[:], in_=slot[:], scalar=BIG, op=ALU.add), reads=[d_slot], writes=[d_slot])
        P.op("dve", lambda e: e.tensor_copy(out=idx[:], in_=slot[:]), reads=[d_slot], writes=[d_idx])


def dispatch_phase(P, H, R, XE, XEC):
    aff, idx = R["aff"], R["idx"]
    with P.scope():
        hx = [P.sb("d_hx%d" % i, [128, HW], BF16) for i in range(3)]
        d_hx = [Dep() for _ in range(3)]
        for n in range(NT):
            i = n % 3
            P.dma("sp", hx[i][:, 0:D], H[n * 128:(n + 1) * 128, :], writes=[d_hx[i]])
            P.op("act", lambda e: e.copy(out=hx[i][:, D:HW], in_=R["aff3"][:, n, :, :].rearrange("p e k -> p (e k)")), reads=[R["d_aff3"]], writes=[d_hx[i]])
            for ex in range(NE):
                dst = XEC[ex] if n < NCTX_T else XE[ex]
                cap = CAP_CTX if n < NCTX_T else CAP_LAT
                P.dma("pool", dst[:, :], hx[i][:, :], reads=[d_hx[i], R["d_idx"]],
                      indirect=dict(out_offset=bass.IndirectOffsetOnAxis(ap=idx[:, n, ex:ex + 1], axis=0), in_offset=None,
                                    bounds_check=P.bounds_reg(cap - 1), oob_is_err=False))


def expert_phase(P, layer, io, XE, XEC, YE, YEC):
    NS = CAP_LAT + CAP_CTX
    with P.scope():
        ident, d_id = make_ident(P)
        wg = P.sb("e_wg", [128, 16, 1024], BF16)
        wu = P.sb("e_wu", [128, 16, 1024], BF16)
        wd = P.sb("e_wd", [128, 8, D], BF16)
        d_wg, d_wu, d_wd = Dep(), Dep(), Dep()
        xin = [P.sb("e_xin%d" % i, [128, HW], BF16) for i in range(2)]
        d_xin = [Dep(), Dep()]
        xeT = P.sb("e_xeT", [128, 16, NS], BF16)
        d_xeT = Dep()
        gts = P.sb("e_gt", [128, 9], F32)
        d_gts = Dep()
        hT = P.sb("e_hT", [128, 8, NS], BF16)
        d_hT = Dep()
        sl = [P.sb("e_sl%d" % i, [128, 512], F32) for i in range(2)]
        d_sl = [Dep(), Dep()]
        yst = [P.sb("e_y%d" % i, [128, D], F32) for i in range(2)]
        d_yst = [Dep(), Dep()]
        pT = P.ps("e_pT", [128, 16, 128], BF16)
        d_pT = Dep()
        pa = [P.ps("e_pa%d" % i, [128, 512], F32) for i in range(2)]
        pu = [P.ps("e_pu%d" % i, [128, 512], F32) for i in range(2)]
        d_pa = [Dep(), Dep()]
        d_pu = [Dep(), Dep()]
        py = [P.ps("e_py%d" % i, [128, 512], F32) for i in range(2)]
        d_py = [Dep(), Dep()]
        nx = 0
        na = 0
        ny = 0
        for ex in range(NE):
            P.dma("pool", wg[:], io["exp_w_gate"][layer, ex].rearrange("(j p) n -> p j n", p=128), writes=[d_wg])
            P.dma("pool", wu[:], io["exp_w_up"][layer, ex].rearrange("(j p) n -> p j n", p=128), writes=[d_wu])
            P.dma("pool", wd[:], io["exp_w_down"][layer, ex].rearrange("(j p) n -> p j n", p=128), writes=[d_wd])
            for s_ in range(9):
                m = 128 if s_ < 8 else CAP_CTX
                i = nx % 2
                nx += 1
                src = XE[ex][s_ * 128:(s_ + 1) * 128, :] if s_ < 8 else XEC[ex][:, :]
                P.dma("sp", xin[i][0:m, :], src, writes=[d_xin[i]])
                P.op("dve", lambda e: e.tensor_reduce(out=gts[0:m, s_:s_ + 1], in_=xin[i][0:m, D + 3 * ex:D + 3 * ex + 3], axis=AX.X, op=ALU.add),
                     reads=[d_xin[i]], writes=[d_gts])
                for kc in range(16):
                    P.op("pe", lambda e: e.transpose(pT[:, kc, 0:m], xin[i][0:m, kc * 128:(kc + 1) * 128], ident[0:m, 0:m]),
                         reads=[d_xin[i], d_id], writes=[d_pT], signal=(kc == 15))
                copy_evac(P, nx, xeT[:, :, s_ * 128:s_ * 128 + m], pT[:, :, 0:m], [d_pT], [d_xeT])
            for fc in range(8):
                for c0 in range(0, NS, 512):
                    cw = min(512, NS - c0)
                    i = na % 2
                    na += 1
                    for kc in range(16):
                        P.op("pe", lambda e: e.matmul(pa[i][:, :cw], lhsT=wg[:, kc, fc * 128:(fc + 1) * 128], rhs=xeT[:, kc, c0:c0 + cw],
                                                      start=(kc == 0), stop=(kc == 15)),
                             reads=[d_wg, d_xeT], writes=[d_pa[i]], signal=(kc == 15))
                    for kc in range(16):
                        P.op("pe", lambda e: e.matmul(pu[i][:, :cw], lhsT=wu[:, kc, fc * 128:(fc + 1) * 128], rhs=xeT[:, kc, c0:c0 + cw],
                                                      start=(kc == 0), stop=(kc == 15)),
                             reads=[d_wu, d_xeT], writes=[d_pu[i]], signal=(kc == 15))
                    P.op("act", lambda e: e.activation(out=sl[i][:, :cw], in_=pa[i][:, :cw], func=AF.Silu), reads=[d_pa[i]], writes=[d_sl[i]])
                    P.op("dve", lambda e: e.tensor_tensor(out=hT[:, fc, c0:c0 + cw], in0=sl[i][:, :cw], in1=pu[i][:, :cw], op=ALU.mult),
                         reads=[d_sl[i], d_pu[i]], writes=[d_hT])
            for s_ in range(9):
                m = 128 if s_ < 8 else CAP_CTX
                yi = s_ % 2
                for cb in range(4):
                    i = ny % 2
                    ny += 1
                    for fc in range(8):
                        P.op("pe", lambda e: e.matmul(py[i][0:m, :], lhsT=hT[:, fc, s_ * 128:s_ * 128 + m], rhs=wd[:, fc, cb * 512:(cb + 1) * 512],
                                                      start=(fc == 0), stop=(fc == 7)),
                             reads=[d_hT, d_wd], writes=[d_py[i]], signal=(fc == 7))
                    P.op("act", lambda e: e.activation(out=yst[yi][0:m, cb * 512:(cb + 1) * 512], in_=py[i][0:m, :], func=AF.Copy,
                                                       scale=gts[0:m, s_:s_ + 1]),
                         reads=[d_py[i], d_gts], writes=[d_yst[yi]])
                dst = YE[ex][s_ * 128:(s_ + 1) * 128, :] if s_ < 8 else YEC[ex][:, :]
                P.dma("sp", dst, yst[yi][0:m, :], reads=[d_yst[yi]])


def combine_norm_phase(P, X, MOD, layer, R, YE, YEC, gate_idx, Xsrc=None):
    idx = R["idx"]
    NB = 4
    with P.scope():
        G = [P.sb("c_G%d" % s_, [128, D], F32) for s_ in range(2)]
        d_G = [Dep(), Dep()]
        for s_ in range(2):
            load_bc(P, "sp", G[s_][:], MOD[layer, s_, gate_idx * D:(gate_idx + 1) * D], d_G[s_])
        acc = [P.sb("c_acc%d" % i, [128, D], F32) for i in range(NB)]
        d_acc = [Dep() for _ in range(NB)]
        xt = [P.sb("c_x%d" % i, [128, D], F32) for i in range(NB)]
        d_x = [Dep() for _ in range(NB)]
        buf = [P.sb("c_buf%d" % i, [128, D], F32) for i in range(6)]
        d_buf = [Dep() for _ in range(6)]
        for b_ in range(6):
            P.op("pool", lambda e: e.memset(buf[b_][:], 0.0), writes=[d_buf[b_]])
        nbuf = 0
        for n0 in range(0, NT, NB):
            grp = list(range(n0, min(NT, n0 + NB)))
            for n in grp:
                i = n % NB
                P.dma("sp", xt[i][:], (X if Xsrc is None else Xsrc)[n * 128:(n + 1) * 128, :], writes=[d_x[i]])
                P.op("dve", lambda e: e.memset(acc[i][:], 0.0), writes=[d_acc[i]])
            for ex in range(NE):
                for n in grp:
                    i = n % NB
                    src = YEC[ex] if n < NCTX_T else YE[ex]
                    cap = CAP_CTX if n < NCTX_T else CAP_LAT
                    kb = nbuf % len(buf)
                    nbuf += 1
                    P.dma("pool", buf[kb][:, :], src[:, :], reads=[R["d_idx"]], writes=[d_buf[kb]],
                          indirect=dict(out_offset=None, in_offset=bass.IndirectOffsetOnAxis(ap=idx[:, n, ex:ex + 1], axis=0),
                                        bounds_check=P.bounds_reg(cap - 1), oob_is_err=False))
                    P.op("dve", lambda e: e.scalar_tensor_tensor(out=acc[i][:], in0=buf[kb][:], scalar=R["sel"][:, n, ex:ex + 1], in1=acc[i][:],
                                                                 op0=ALU.mult, op1=ALU.add),
                         reads=[d_buf[kb], d_acc[i], R["d_sel"]], writes=[d_acc[i]])
            for n in grp:
                i = n % NB
                s_ = 1 if n < NCTX_T else 0
                P.op("dve", lambda e: e.tensor_mul(out=acc[i][:], in0=acc[i][:], in1=G[s_][:]), reads=[d_acc[i], d_G[s_]], writes=[d_acc[i]])
                P.op("dve", lambda e: e.tensor_add(out=xt[i][:], in0=xt[i][:], in1=acc[i][:]), reads=[d_acc[i], d_x[i]], writes=[d_x[i]])
                P.dma("sp", X[n * 128:(n + 1) * 128, :], xt[i][:], reads=[d_x[i]])


RH, RDK, RDV = 8, 256, 512


def retention_scan(P, dr, PROJ, decay_ap, consts, YF, Y):
    order = list(range(NT)) if dr == 0 else [1, 0] + list(range(NT - 1, NCTX_T - 1, -1))
    with P.scope():
        ident, d_id = make_ident(P)
        lgb = P.sb("s_lgb", [128, RH], F32)
        d_lgb = Dep()
        load_bc(P, "sp", lgb[:], decay_ap[dr], d_lgb)
        P.op("act", lambda e: e.activation(out=lgb[:], in_=lgb[:], func=AF.Exp), reads=[d_lgb], writes=[d_lgb])
        P.op("dve", lambda e: e.tensor_single_scalar(out=lgb[:], in_=lgb[:], scalar=-1.0, op=ALU.mult), reads=[d_lgb], writes=[d_lgb])
        Em = P.sb("s_E", [128, 128], F32)
        Mm = P.sb("s_M", [128, 128], F32)
        qpos = P.sb("s_qpos", [128, 128], F32)
        kpos = P.sb("s_kpos", [128, 1], F32)
        d_c = Dep()
        P.dma("sp", Em[:], consts["c_tri"][2 * dr], wshared=[d_c])
        P.dma("sp", Mm[:], consts["c_tri"][2 * dr + 1], wshared=[d_c])
        load_bc(P, "sp", qpos[:], consts["c_qpos"][dr], Dep())
        P.dma("sp", kpos[:], consts["c_kpos"][dr], wshared=[d_c])
        P.barrier()
        Dm = P.sb("s_Dm", [128, RH, 128], F32)
        qdT = P.sb("s_qdT", [128, RH, 128], BF16)
        kdec = P.sb("s_kdec", [128, RH], F32)
        cd = P.sb("s_cd", [128, RH], F32)
        d_tab = Dep()
        for h in range(RH):
            P.op("act", lambda e: e.activation(out=Dm[:, h, :], in_=Em[:], func=AF.Exp, scale=lgb[:, h:h + 1]), reads=[d_lgb], writes=[d_tab])
            P.op("dve", lambda e: e.tensor_tensor(out=Dm[:, h, :], in0=Dm[:, h, :], in1=Mm[:], op=ALU.mult), reads=[d_tab], writes=[d_tab])
            P.op("act", lambda e: e.activation(out=qdT[:, h, :], in_=qpos[:], func=AF.Exp, scale=lgb[:, h:h + 1]), reads=[d_lgb], writes=[d_tab])
        P.op("dve", lambda e: e.tensor_scalar(out=kdec[:], in0=lgb[:], scalar1=kpos[:, 0:1], scalar2=None, op0=ALU.mult), reads=[d_lgb], writes=[d_tab])
        P.op("act", lambda e: e.activation(out=kdec[:], in_=kdec[:], func=AF.Exp), reads=[d_tab], writes=[d_tab])
        P.op("act", lambda e: e.activation(out=cd[:], in_=lgb[:], func=AF.Exp, scale=128.0), reads=[d_lgb], writes=[d_tab])
        S = P.sb("s_S", [128, RH, 2, RDV], F32)
        Sb = P.sb("s_Sb", [128, RH, 2, RDV], BF16)
        d_S = [Dep() for _ in range(RH)]
        d_Sb = [Dep() for _ in range(RH)]
        for h in range(RH):
            P.op("pool", lambda e: e.memset(S[:, h], 0.0), writes=[d_S[h]])
            P.op("pool", lambda e: e.memset(Sb[:, h], 0.0), writes=[d_Sb[h]])
        qk = P.sb("s_qk", [128, 4096], BF16)
        v = P.sb("s_v", [128, 4096], BF16)
        g = P.sb("s_g", [128, 4096], BF16)
        d_qk, d_v, d_g = Dep(), Dep(), Dep()
        rq = P.sb("s_rq", [128, 256], F32)
        rk = P.sb("s_rk", [128, 256], F32)
        d_rq, d_rk = Dep(), Dep()
        qr = P.sb("s_qr", [128, 2048], BF16)
        kr = P.sb("s_kr", [128, 2048], BF16)
        d_qr, d_kr = Dep(), Dep()
        ta = [P.sb("s_ta%d" % i, [128, RH, 2, 64], F32) for i in range(2)]
        tb = [P.sb("s_tb%d" % i, [128, RH, 2, 64], F32) for i in range(2)]
        d_ta = [Dep(), Dep()]
        d_tb = [Dep(), Dep()]
        qT = P.sb("s_qT", [128, 16, 128], BF16)
        kT = P.sb("s_kT", [128, 16, 128], BF16)
        qTd = P.sb("s_qTd", [128, 16, 128], BF16)
        k2 = P.sb("s_k2", [128, 2048], BF16)
        d_qT, d_kT, d_qTd, d_k2 = Dep(), Dep(), Dep(), Dep()
        sTm = [P.sb("s_sTm%d" % i, [128, 128], BF16) for i in range(2)]
        d_sTm = [Dep(), Dep()]
        osb = P.sb("s_o", [128, RH, RDV], F32)
        d_o = Dep()
        junk = P.sb("s_junk", [128, RDV], F32)
        d_junk = Dep()
        s1 = P.sb("s_s1", [128, RH], F32)
        s2 = P.sb("s_s2", [128, RH], F32)
        mean = P.sb("s_mean", [128, RH], F32)
        rstd = P.sb("s_rstd", [128, RH], F32)
        nmr = P.sb("s_nmr", [128, RH], F32)
        d_s1, d_s2, d_st = Dep(), Dep(), Dep()
        sg = P.sb("s_sg", [128, 4096], BF16)
        d_sg = Dep()
        if dr == 1:
            yf = P.sb("s_yf", [128, 4096], F32)
            d_yf = Dep()
            yb = P.sb("s_yb", [128, 4096], BF16)
            d_yb = Dep()
        pT = P.ps("s_pT", [128, 16, 128], BF16)
        d_pT = Dep()
        psT = P.ps("s_psT", [128, 128], F32)
        d_psT = Dep()
        po = [P.ps("s_po%d" % i, [128, RDV], F32) for i in range(2)]
        d_po = [Dep(), Dep()]
        pS = [P.ps("s_pS%d" % i, [128, RDV], F32) for i in range(2)]
        d_pS = [Dep(), Dep()]

        def rope(eng, i, src, tab, dst, d_tab_, d_dst):
            xv = src.rearrange("p (h a b d) -> p h a b d", h=RH, a=2, b=2)
            ov = dst[:].rearrange("p (h a b d) -> p h a b d", h=RH, a=2, b=2)
            tv = tab[:].rearrange("p (a c d) -> p a c d", a=2, c=2)
            cos = tv[:, :, 0, :].unsqueeze(1).to_broadcast([128, RH, 2, 64])
            sin = tv[:, :, 1, :].unsqueeze(1).to_broadcast([128, RH, 2, 64])
            x1, x2 = xv[:, :, :, 0, :], xv[:, :, :, 1, :]
            A, B = ta[i], tb[i]
            P.op(eng, lambda e: e.tensor_tensor(out=A[:], in0=x1, in1=cos, op=ALU.mult), reads=[d_qk, d_tab_], writes=[d_ta[i]])
            P.op(eng, lambda e: e.tensor_tensor(out=B[:], in0=x2, in1=sin, op=ALU.mult), reads=[d_qk, d_tab_], writes=[d_tb[i]])
            P.op(eng, lambda e: e.tensor_tensor(out=ov[:, :, :, 0, :], in0=A[:], in1=B[:], op=ALU.subtract), reads=[d_ta[i], d_tb[i]], writes=[d_dst])
            P.op(eng, lambda e: e.tensor_tensor(out=A[:], in0=x1, in1=sin, op=ALU.mult), reads=[d_qk, d_tab_], writes=[d_ta[i]])
            P.op(eng, lambda e: e.tensor_tensor(out=B[:], in0=x2, in1=cos, op=ALU.mult), reads=[d_qk, d_tab_], writes=[d_tb[i]])
            P.op(eng, lambda e: e.tensor_tensor(out=ov[:, :, :, 1, :], in0=A[:], in1=B[:], op=ALU.add), reads=[d_ta[i], d_tb[i]], writes=[d_dst])

        for t in order:
            rows = slice(t * 128, (t + 1) * 128)
            P.dma("sp", qk[:], PROJ[0][rows, :], writes=[d_qk])
            P.dma("sp", v[:], PROJ[1][rows, :], writes=[d_v])
            P.dma("sp", g[:], PROJ[2 + dr][rows, :], writes=[d_g])
            if dr == 1:
                P.dma("sp", yf[:], YF[rows, :], writes=[d_yf])
            if t >= NCTX_T:
                lr = slice((t - NCTX_T) * 128, (t - NCTX_T + 1) * 128)
                P.dma("sp", rq[:], consts["c_ropeq"][lr, :], writes=[d_rq])
                P.dma("sp", rk[:], consts["c_ropek"][lr, :], writes=[d_rk])
                rope("dve", 0, qk[:, 0:2048], rq, qr, d_rq, d_qr)
                rope("pool", 1, qk[:, 2048:4096], rk, kr, d_rk, d_kr)
            else:
                P.op("act", lambda e: e.mul(out=qr[:], in_=qk[:, 0:2048], mul=1.0 / 16.0), reads=[d_qk], writes=[d_qr])
                P.op("pool", lambda e: e.tensor_copy(out=kr[:], in_=qk[:, 2048:4096]), reads=[d_qk], writes=[d_kr])
            P.op("act", lambda e: e.activation(out=sg[:], in_=g[:], func=AF.Silu), reads=[d_g], writes=[d_sg])
            for (src_, d_src, dstT, d_dstT) in ((qr, d_qr, qT, d_qT), (kr, d_kr, kT, d_kT)):
                for c in range(16):
                    P.op("pe", lambda e: e.transpose(pT[:, c, :], src_[:, c * 128:(c + 1) * 128], ident[:]),
                         reads=[d_src, d_id], writes=[d_pT], signal=(c == 15))
                P.op("act", lambda e: e.copy(out=dstT[:], in_=pT[:]), reads=[d_pT], writes=[d_dstT])
            P.op("pool", lambda e: e.tensor_tensor(out=k2[:].rearrange("p (h d) -> p h d", h=RH), in0=kr[:].rearrange("p (h d) -> p h d", h=RH),
                                                   in1=kdec[:].unsqueeze(2).to_broadcast([128, RH, RDK]), op=ALU.mult),
                 reads=[d_kr, d_tab], writes=[d_k2])
            P.op("dve", lambda e: e.tensor_tensor(out=qTd[:].rearrange("p (h c) t -> p h c t", c=2), in0=qT[:].rearrange("p (h c) t -> p h c t", c=2),
                                                  in1=qdT[:].unsqueeze(2).to_broadcast([128, RH, 2, 128]), op=ALU.mult),
                 reads=[d_qT, d_tab], writes=[d_qTd])
            for h in range(RH):
                i = h % 2
                for c2 in range(2):
                    P.op("pe", lambda e: e.matmul(psT[:], lhsT=kT[:, h * 2 + c2, :], rhs=qT[:, h * 2 + c2, :], start=(c2 == 0), stop=(c2 == 1)),
                         reads=[d_kT, d_qT], writes=[d_psT], signal=(c2 == 1))
                P.op("dve", lambda e: e.tensor_tensor(out=sTm[i][:], in0=psT[:], in1=Dm[:, h, :], op=ALU.mult), reads=[d_psT, d_tab], writes=[d_sTm[i]])
                for c2 in range(2):
                    P.op("pe", lambda e: e.matmul(po[i][:], lhsT=qTd[:, h * 2 + c2, :], rhs=Sb[:, h, c2, :], start=(c2 == 0), stop=False),
                         reads=[d_qTd, d_Sb[h]], writes=[d_po[i]], signal=False)
                P.op("pe", lambda e: e.matmul(po[i][:], lhsT=sTm[i][:], rhs=v[:, h * RDV:(h + 1) * RDV], start=False, stop=True),
                     reads=[d_sTm[i], d_v], writes=[d_po[i]])
                P.op("act", lambda e: e.activation(out=osb[:, h, :], in_=po[i][:], func=AF.Copy, accum_out=s1[:, h:h + 1]),
                     reads=[d_po[i]], writes=[d_o, d_s1])
                P.op("act", lambda e: e.activation(out=junk[:], in_=po[i][:], func=AF.Square, accum_out=s2[:, h:h + 1]),
                     reads=[d_po[i]], writes=[d_junk, d_s2])
                for c2 in range(2):
                    P.op("pe", lambda e: e.matmul(pS[c2][:], lhsT=k2[:, h * RDK + c2 * 128:h * RDK + (c2 + 1) * 128], rhs=v[:, h * RDV:(h + 1) * RDV],
                                                  start=True, stop=True), reads=[d_k2, d_v], writes=[d_pS[c2]])
                    P.op("dve", lambda e: e.scalar_tensor_tensor(out=S[:, h, c2, :], in0=S[:, h, c2, :], scalar=cd[:, h:h + 1], in1=pS[c2][:],
                                                                 op0=ALU.mult, op1=ALU.add),
                         reads=[d_pS[c2], d_tab], writes=[d_S[h]])
                P.op("pool", lambda e: e.tensor_copy(out=Sb[:, h], in_=S[:, h]), reads=[d_S[h]], writes=[d_Sb[h]])
            P.op("dve", lambda e: e.tensor_single_scalar(out=mean[:], in_=s1[:], scalar=1.0 / RDV, op=ALU.mult), reads=[d_s1], writes=[d_st])
            P.op("dve", lambda e: e.tensor_tensor(out=nmr[:], in0=mean[:], in1=mean[:], op=ALU.mult), reads=[d_st], writes=[d_st])
            P.op("dve", lambda e: e.scalar_tensor_tensor(out=rstd[:], in0=s2[:], scalar=1.0 / RDV, in1=nmr[:], op0=ALU.mult, op1=ALU.subtract),
                 reads=[d_s2, d_st], writes=[d_st])
            P.op("act", lambda e: e.activation(out=rstd[:], in_=rstd[:], func=AF.Sqrt, bias=EPS, scale=1.0), reads=[d_st], writes=[d_st])
            P.op("dve", lambda e: e.reciprocal(out=rstd[:], in_=rstd[:]), reads=[d_st], writes=[d_st])
            P.op("dve", lambda e: e.tensor_tensor(out=osb[:], in0=osb[:], in1=mean[:].unsqueeze(2).to_broadcast([128, RH, RDV]), op=ALU.subtract),
                 reads=[d_o, d_st], writes=[d_o])
            P.op("dve", lambda e: e.tensor_tensor(out=osb[:], in0=osb[:], in1=rstd[:].unsqueeze(2).to_broadcast([128, RH, RDV]), op=ALU.mult),
                 reads=[d_o, d_st], writes=[d_o])
            of = osb[:].rearrange("p h d -> p (h d)")
            P.op("pool", lambda e: e.tensor_tensor(out=of, in0=of, in1=sg[:], op=ALU.mult), reads=[d_o, d_sg], writes=[d_o])
            if dr == 0:
                P.dma("sp", YF[rows, :], of, reads=[d_o])
            else:
                P.op("dve", lambda e: e.tensor_tensor(out=yb[:], in0=of, in1=yf[:], op=ALU.add), reads=[d_o, d_yf], writes=[d_yb])
                P.dma("sp", Y[rows, :], yb[:], reads=[d_yb])


AH, AKV, AHD = 16, 4, 128


def rope_a(P, eng, src, tab, dst, nh, A, B, d_src, d_tab_, d_A, d_B, d_dst):
    xv = src.rearrange("p (h a b d) -> p h a b d", h=nh, a=2, b=2)
    ov = dst.rearrange("p (h a b d) -> p h a b d", h=nh, a=2, b=2)
    tv = tab[:].rearrange("p (a c d) -> p a c d", a=2, c=2)
    cos = tv[:, :, 0, :].unsqueeze(1).to_broadcast([128, nh, 2, 32])
    sin = tv[:, :, 1, :].unsqueeze(1).to_broadcast([128, nh, 2, 32])
    x1, x2 = xv[:, :, :, 0, :], xv[:, :, :, 1, :]
    Av = A[:, 0:nh * 64].rearrange("p (h a d) -> p h a d", h=nh, a=2)
    Bv = B[:, 0:nh * 64].rearrange("p (h a d) -> p h a d", h=nh, a=2)
    P.op(eng, lambda e: e.tensor_tensor(out=Av, in0=x1, in1=cos, op=ALU.mult), reads=[d_src, d_tab_], writes=[d_A])
    P.op(eng, lambda e: e.tensor_tensor(out=Bv, in0=x2, in1=sin, op=ALU.mult), reads=[d_src, d_tab_], writes=[d_B])
    P.op(eng, lambda e: e.tensor_tensor(out=ov[:, :, :, 0, :], in0=Av, in1=Bv, op=ALU.subtract), reads=[d_A, d_B], writes=[d_dst])
    P.op(eng, lambda e: e.tensor_tensor(out=Av, in0=x1, in1=sin, op=ALU.mult), reads=[d_src, d_tab_], writes=[d_A])
    P.op(eng, lambda e: e.tensor_tensor(out=Bv, in0=x2, in1=cos, op=ALU.mult), reads=[d_src, d_tab_], writes=[d_B])
    P.op(eng, lambda e: e.tensor_tensor(out=ov[:, :, :, 1, :], in0=Av, in1=Bv, op=ALU.add), reads=[d_A, d_B], writes=[d_dst])


def attn_prep(P, QKV, consts, KT):
    with P.scope():
        ident, d_id = make_ident(P)
        kin = P.sb("a_kin", [128, 512], BF16)
        kr = P.sb("a_kr", [128, 512], BF16)
        tab = P.sb("a_tab", [128, 128], F32)
        A = P.sb("a_A", [128, 1024], F32)
        B = P.sb("a_B", [128, 1024], F32)
        kT = P.sb("a_kT", [128, AKV, 128], BF16)
        pT = P.ps("a_pT", [128, AKV, 128], BF16)
        d_kin, d_kr, d_tab, d_A, d_B, d_kT, d_pT = (Dep() for _ in range(7))
        for t in range(NT):
            rows = slice(t * 128, (t + 1) * 128)
            P.dma("sp", kin[:], QKV[rows, 2048:2560], writes=[d_kin])
            if t >= NCTX_T:
                lr = slice((t - NCTX_T) * 128, (t - NCTX_T + 1) * 128)
                P.dma("sp", tab[:], consts["c_ropea"][lr, :], writes=[d_tab])
                rope_a(P, "dve", kin[:], tab, kr[:], AKV, A, B, d_kin, d_tab, d_A, d_B, d_kr)
            else:
                P.op("dve", lambda e: e.tensor_copy(out=kr[:], in_=kin[:]), reads=[d_kin], writes=[d_kr])
            for c in range(AKV):
                P.op("pe", lambda e: e.transpose(pT[:, c, :], kr[:, c * 128:(c + 1) * 128], ident[:]), reads=[d_kr, d_id], writes=[d_pT],
                     signal=(c == AKV - 1))
            P.op("act", lambda e: e.copy(out=kT[:], in_=pT[:]), reads=[d_pT], writes=[d_kT])
            P.dma("sp", KT[t], kT[:].rearrange("p c t -> p (c t)"), reads=[d_kT])


def attn_main(P, QKV, sink_ap, consts, KT, Y):
    SC = float(AHD) ** -0.5
    with P.scope():
        ident, d_id = make_ident(P)
        sk = P.sb("b_sk", [128, AH], F32)
        mprev = P.sb("b_mp", [128, 128], F32)
        mnext = P.sb("b_mn", [128, 128], F32)
        load_bc(P, "sp", sk[:], sink_ap, Dep())
        P.dma("sp", mprev[:], consts["c_tri"][1])
        P.dma("sp", mnext[:], consts["c_tri"][3])
        P.barrier()
        qin = P.sb("b_qin", [128, 2048], BF16)
        qr = P.sb("b_qr", [128, 2048], BF16)
        tab = P.sb("b_tab", [128, 128], F32)
        A = P.sb("b_A", [128, 1024], F32)
        B = P.sb("b_B", [128, 1024], F32)
        qT = P.sb("b_qT", [128, AH, 128], BF16)
        kTa = P.sb("b_kTa", [128, 5, AKV * 128], BF16)
        va = P.sb("b_va", [128, 5, 512], BF16)
        d_qin, d_qr, d_tab, d_A, d_B, d_qT, d_kTa, d_va = (Dep() for _ in range(8))
        pr = [P.sb("b_p%d" % i, [128, 5, 128], BF16) for i in range(2)]
        d_pr = [Dep(), Dep()]
        pTs = [P.sb("b_pTs%d" % i, [128, 5, 128], BF16) for i in range(2)]
        d_pTs = [Dep(), Dep()]
        st = [P.sb("b_st%d" % i, [128, 8], F32) for i in range(2)]
        d_st = [Dep(), Dep()]
        y = P.sb("b_y", [128, 2048], BF16)
        d_y = Dep()
        pq = P.ps("b_pq", [128, 16, 128], BF16)
        d_pq = Dep()
        pS = [P.ps("b_pS%d" % i, [128, 5, 128], F32) for i in range(1)]
        d_pS = [Dep()]
        pP = P.ps("b_pP", [128, 5, 128], BF16)
        d_pP = Dep()
        pO = [P.ps("b_pO%d" % i, [128, 128], F32) for i in range(2)]
        d_pO = [Dep(), Dep()]
        for t in range(NT):
            rows = slice(t * 128, (t + 1) * 128)
            isc = t < NCTX_T
            keys = [(0, 0), (1, 0)]
            if not isc:
                if t - 1 >= NCTX_T:
                    keys.append((t - 1, 1))
                keys.append((t, 0))
                if t + 1 < NT:
                    keys.append((t + 1, 2))
            nk = len(keys)
            P.dma("sp", qin[:], QKV[rows, 0:2048], writes=[d_qin])
            for ki, (kt, _) in enumerate(keys):
                P.dma("sp", kTa[:, ki, :], KT[kt], writes=[d_kTa] if ki == 0 else [], wshared=[d_kTa] if ki else [])
                P.dma("sp", va[:, ki, :], QKV[kt * 128:(kt + 1) * 128, 2560:3072], writes=[d_va] if ki == 0 else [], wshared=[d_va] if ki else [])
            if not isc:
                lr = slice((t - NCTX_T) * 128, (t - NCTX_T + 1) * 128)
                P.dma("sp", tab[:], consts["c_ropea"][lr, :], writes=[d_tab])
                rope_a(P, "pool", qin[:], tab, qr[:], AH, A, B, d_qin, d_tab, d_A, d_B, d_qr)
            else:
                P.op("pool", lambda e: e.tensor_copy(out=qr[:], in_=qin[:]), reads=[d_qin], writes=[d_qr])
            for c in range(AH):
                P.op("pe", lambda e: e.transpose(pq[:, c, :], qr[:, c * 128:(c + 1) * 128], ident[:]), reads=[d_qr, d_id], writes=[d_pq],
                     signal=(c == AH - 1))
            P.op("act", lambda e: e.copy(out=qT[:], in_=pq[:]), reads=[d_pq], writes=[d_qT])
            for hd in range(AH):
                i = hd % 2
                kv = hd // (AH // AKV)
                S_ = pS[0]
                for ki in range(nk):
                    P.op("pe", lambda e: e.matmul(S_[:, ki, :], lhsT=qT[:, hd, :], rhs=kTa[:, ki, kv * 128:(kv + 1) * 128], start=True, stop=True),
                         reads=[d_qT, d_kTa], writes=[d_pS[0]], signal=(ki == nk - 1))
                mx, negm, den, es, rden = (st[i][:, k:k + 1] for k in range(5))
                P.op("dve", lambda e: e.tensor_reduce(out=mx, in_=S_[:, 0:nk, :].rearrange("p k t -> p (k t)"), axis=AX.X, op=ALU.max),
                     reads=[d_pS[0]], writes=[d_st[i]])
                P.op("dve", lambda e: e.tensor_scalar(out=mx, in0=mx, scalar1=SC, scalar2=sk[:, hd:hd + 1], op0=ALU.mult, op1=ALU.max),
                     reads=[d_st[i]], writes=[d_st[i]])
                P.op("dve", lambda e: e.tensor_single_scalar(out=negm, in_=mx, scalar=-1.0, op=ALU.mult), reads=[d_st[i]], writes=[d_st[i]])
                P.op("act", lambda e: e.activation(out=pr[i][:, 0:nk, :], in_=S_[:, 0:nk, :], func=AF.Exp, bias=negm, scale=SC),
                     reads=[d_pS[0], d_st[i]], writes=[d_pr[i]])
                P.op("act", lambda e: e.activation(out=es, in_=sk[:, hd:hd + 1], func=AF.Exp, bias=negm, scale=1.0), reads=[d_st[i]], writes=[d_st[i]])
                for ki, (kt, mk) in enumerate(keys):
                    if mk:
                        mm = mprev if mk == 1 else mnext
                        P.op("dve", lambda e: e.tensor_tensor(out=pr[i][:, ki, :], in0=pr[i][:, ki, :], in1=mm[:], op=ALU.mult),
                             reads=[d_pr[i]], writes=[d_pr[i]])
                P.op("dve", lambda e: e.tensor_reduce(out=den, in_=pr[i][:, 0:nk, :].rearrange("p k t -> p (k t)"), axis=AX.X, op=ALU.add),
                     reads=[d_pr[i]], writes=[d_st[i]])
                P.op("dve", lambda e: e.tensor_tensor(out=den, in0=den, in1=es, op=ALU.add), reads=[d_st[i]], writes=[d_st[i]])
                P.op("dve", lambda e: e.reciprocal(out=rden, in_=den), reads=[d_st[i]], writes=[d_st[i]])
                for ki in range(nk):
                    P.op("pe", lambda e: e.transpose(pP[:, ki, :], pr[i][:, ki, :], ident[:]), reads=[d_pr[i], d_id], writes=[d_pP], signal=(ki == nk - 1))
                P.op("act", lambda e: e.copy(out=pTs[i][:, 0:nk, :], in_=pP[:, 0:nk, :]), reads=[d_pP], writes=[d_pTs[i]])
                for ki in range(nk):
                    P.op("pe", lambda e: e.matmul(pO[i][:], lhsT=pTs[i][:, ki, :], rhs=va[:, ki, kv * 128:(kv + 1) * 128], start=(ki == 0), stop=(ki == nk - 1)),
                         reads=[d_pTs[i], d_va], writes=[d_pO[i]], signal=(ki == nk - 1))
                P.op("act", lambda e: e.activation(out=y[:, hd * 128:(hd + 1) * 128], in_=pO[i][:], func=AF.Copy, scale=rden),
                     reads=[d_pO[i], d_st[i]], writes=[d_y])
            P.dma("sp", Y[rows, 0:2048], y[:], reads=[d_y])


def fourier_pos(P, AB, consts, Z):
    CW = 256
    with P.scope():
        for (name, t0, ntl) in (("c_dftc", 0, NCTX_T), ("c_dft", NCTX_T, NT - NCTX_T)):
            Dmat = consts[name]
            with P.scope():
                Asl = P.sb("f_A", [128, ntl, CW], BF16)
                Bsl = P.sb("f_B", [128, ntl, CW], BF16)
                d_A, d_B = Dep(), Dep()
                Dc = [P.sb("f_Dc%d" % i, [128, ntl, 128], BF16) for i in range(2)]
                Ds = [P.sb("f_Ds%d" % i, [128, ntl, 128], BF16) for i in range(2)]
                d_Dc = [Dep(), Dep()]
                d_Ds = [Dep(), Dep()]
                zs = [P.sb("f_z%d" % i, [128, CW], BF16) for i in range(2)]
                d_zs = [Dep(), Dep()]
                pz = [P.ps("f_pz%d" % i, [128, CW], F32) for i in range(2)]
                d_pz = [Dep(), Dep()]
                r0, r1 = t0 * 128, (t0 + ntl) * 128
                n = 0
                for cb in range(D // CW):
                    Av = AB[r0:r1, cb * CW:(cb + 1) * CW].rearrange("(mt p) c -> p mt c", p=128)
                    Bv = AB[r0:r1, D + cb * CW:D + (cb + 1) * CW].rearrange("(mt p) c -> p mt c", p=128)
                    for m0 in range(0, ntl, 8):
                        m1 = min(ntl, m0 + 8)
                        P.dma("sp", Asl[:, m0:m1, :], Av[:, m0:m1, :], writes=[d_A] if m0 == 0 else [], wshared=[d_A] if m0 else [])
                        P.dma("sp", Bsl[:, m0:m1, :], Bv[:, m0:m1, :], writes=[d_B] if m0 == 0 else [], wshared=[d_B] if m0 else [])
                    for nt in range(ntl):
                        i = n % 2
                        n += 1
                        P.dma("pool", Dc[i][:], Dmat[0, nt], writes=[d_Dc[i]])
                        P.dma("pool", Ds[i][:], Dmat[1, nt], writes=[d_Ds[i]])
                        for mt in range(ntl):
                            P.op("pe", lambda e: e.matmul(pz[i][:], lhsT=Dc[i][:, mt, :], rhs=Asl[:, mt, :], start=(mt == 0), stop=False),
                                 reads=[d_Dc[i], d_A], writes=[d_pz[i]], signal=False)
                        for mt in range(ntl):
                            P.op("pe", lambda e: e.matmul(pz[i][:], lhsT=Ds[i][:, mt, :], rhs=Bsl[:, mt, :], start=False, stop=(mt == ntl - 1)),
                                 reads=[d_Ds[i], d_B], writes=[d_pz[i]], signal=(mt == ntl - 1))
                        copy_evac(P, n, zs[i][:], pz[i][:], [d_pz[i]], [d_zs[i]])
                        P.dma("sp", Z[(t0 + nt) * 128:(t0 + nt + 1) * 128, cb * CW:(cb + 1) * CW], zs[i][:], reads=[d_zs[i]])


WEIGHT_NAMES = ["w_mod", "b_mod", "norm_gain", "final_gain", "ret_w_in", "ret_w_out", "ret_decay", "win_w_qkv",
                "win_w_o", "win_sink", "fno_w_o", "router_w", "exp_w_gate", "exp_w_up", "exp_w_down"]
WEIGHT_SHAPES = {
    "w_mod": [4, 2048, 12288], "b_mod": [4, 12288], "norm_gain": [4, 2, 2048], "final_gain": [2048],
    "ret_w_in": [2, 2048, 16384], "ret_w_out": [2, 4096, 2048], "ret_decay": [2, 2, 8],
    "win_w_qkv": [1, 2048, 3072], "win_w_o": [1, 2048, 2048], "win_sink": [1, 16], "fno_w_o": [1, 2048, 2048],
    "router_w": [4, 2048, 16], "exp_w_gate": [4, 16, 2048, 1024], "exp_w_up": [4, 16, 2048, 1024],
    "exp_w_down": [4, 16, 1024, 2048],
}


def const_shapes():
    TL = (NT - NCTX_T) * 128
    return {"c_tri": [4, 128, 128], "c_qpos": [2, 128], "c_kpos": [2, 128, 1], "c_ropeq": [TL, 256], "c_ropek": [TL, 256],
            "c_ropea": [TL, 128], "c_fc": [D, 2 * D], "c_dft": [2, NT - NCTX_T, 128, NT - NCTX_T, 128],
            "c_dftc": [2, NCTX_T, 128, NCTX_T, 128]}


def make_consts(used=None):
    TL = (NT - NCTX_T) * 128
    i = np.arange(128)[None, :].astype(np.float64)
    j = np.arange(128)[:, None].astype(np.float64)
    tri = np.stack([np.maximum(i - j, 0), (i >= j).astype(np.float64), np.maximum(j - i, 0), (j >= i).astype(np.float64)])
    qpos = np.stack([np.arange(128) + 1.0, 128.0 - np.arange(128)])
    kpos = np.stack([127.0 - np.arange(128), np.arange(128) * 1.0])[:, :, None]
    t = np.arange(TL)
    row, col = t // 64, t % 64

    def rope_tab(half):
        inv = 10000.0 ** (-np.arange(half, dtype=np.float64) / half)
        out = np.zeros((TL, 2, 2, half))
        for a, pos in enumerate((row, col)):
            ang = (pos[:, None].astype(np.float32) * inv[None, :].astype(np.float32)).astype(np.float64)
            out[:, a, 0, :] = np.cos(ang)
            out[:, a, 1, :] = np.sin(ang)
        return out.reshape(TL, -1)
    r64 = rope_tab(64)
    c = {"c_tri": tri, "c_qpos": qpos, "c_kpos": kpos, "c_ropeq": r64 / 16.0, "c_ropek": r64, "c_ropea": rope_tab(32)}
    c = {k: np.ascontiguousarray(v.astype(np.float32)) for k, v in c.items()}
    if used is None or "c_fc" in used:
        GC = 512
        ang = 2.0 * np.pi * ((np.arange(GC)[:, None] * np.arange(GC)[None, :]) % GC) / GC
        fc = np.zeros((D, 2 * D), np.float32)
        for g_ in range(D // GC):
            fc[g_ * GC:(g_ + 1) * GC, g_ * GC:(g_ + 1) * GC] = np.cos(ang)
            fc[g_ * GC:(g_ + 1) * GC, D + g_ * GC:D + (g_ + 1) * GC] = np.sin(ang)
        c["c_fc"] = fc

        def dft(n_):
            import ml_dtypes
            k_ = np.arange(n_, dtype=np.int64)
            ang_ = 2.0 * np.pi * ((k_[:, None] * k_[None, :]) % n_) / n_
            sc = 1.0 / np.sqrt(n_ * float(GC))
            nt_ = n_ // 128
            out = np.empty((2, nt_, 128, nt_, 128), ml_dtypes.bfloat16)
            for kind_, mat in enumerate((np.cos(ang_) * sc, -np.sin(ang_) * sc)):
                out[kind_] = mat.reshape(nt_, 128, nt_, 128).transpose(2, 1, 0, 3).astype(ml_dtypes.bfloat16)
            return out
        c["c_dft"] = dft((NT - NCTX_T) * 128)
        c["c_dftc"] = dft(NCTX_T * 128)
    return c


def build(stop_after=None, dumps=(), skip_mixer=False, n_layers=4, start_layer=0):
    nc = bass.Bass("TRN2", target_bir_lowering=False)
    io = {}
    io["x_in"] = nc.dram_tensor("x_in", [T, D], F32, kind="ExternalInput").ap()
    io["cvec"] = nc.dram_tensor("cvec", [2, D], F32, kind="ExternalInput").ap()

    class LazyIO(dict):
        def __missing__(self, k):
            shp = WEIGHT_SHAPES[k] if k in WEIGHT_SHAPES else const_shapes()[k]
            self[k] = nc.dram_tensor(k, shp, BF16 if k.startswith("c_dft") else F32, kind="ExternalInput").ap()
            return self[k]
    io = LazyIO(io)
    out = nc.dram_tensor("out", [T - NCTX_T * 128, D], F32, kind="ExternalOutput").ap()

    def scratch(name, shape, dtype):
        kind = "ExternalOutput" if name in dumps else "Internal"
        return nc.dram_tensor(name, list(shape), dtype, kind=kind).ap()

    P = Prog(nc)
    X = scratch("X", [T, D], F32)
    MOD = scratch("MOD", [4, 2, 6 * D], F32)
    H = scratch("H", [T, D], BF16)
    PROJ = [scratch("PROJ%d" % i, [T, 4096], BF16) for i in range(4)]
    MIX = scratch("MIX", [T, D], F32)
    YF = scratch("YF", [T, 4096], F32)
    Y = scratch("Y", [T, 4096], BF16)
    KT = scratch("KT", [NT, 128, AKV * 128], BF16)
    LOGITS = scratch("LOGITS", [T, NE], F32)
    XE = [scratch("XE%d" % i, [CAP_LAT, HW], BF16) for i in range(NE)]
    XEC = [scratch("XEC%d" % i, [CAP_CTX, HW], BF16) for i in range(NE)]
    YE = [scratch("YE%d" % i, [CAP_LAT, D], F32) for i in range(NE)]
    YEC = [scratch("YEC%d" % i, [CAP_CTX, D], F32) for i in range(NE)]
    IDXD = scratch("IDXD", [128, NT * NE], I32)
    AFFD = scratch("AFFD", [128, NT * NE], F32)

    def rows_dst(ap):
        return lambda t, c0, cw: ap[t * 128:(t + 1) * 128, c0:c0 + cw]

    def proj_dst(t, c0, cw):
        return PROJ[c0 // 4096][t * 128:(t + 1) * 128, c0 % 4096:c0 % 4096 + cw]

    def done(name):
        return stop_after == name

    def finish():
        P.barrier()
        P.close()
        return nc, io

    all_tiles = list(range(NT))
    mod_phase(P, io["cvec"], io["w_mod"], io["b_mod"], MOD)
    if done("mod"):
        return finish()
    x_valid = False
    for layer in range(start_layer, n_layers):
        kind, j = layer % 3, layer // 3
        Xs = None if x_valid else io["x_in"]
        if not skip_mixer:
            norm_phase(P, X, MOD, layer, 0, io["norm_gain"][layer, 0], H, Xsrc=Xs)
            if done("norm1_%d" % layer):
                return finish()
            if kind == 0:
                project(P, H, D, io["ret_w_in"][j], 16384, proj_dst, BF16, all_tiles, "pj")
                if done("proj_%d" % layer):
                    return finish()
                retention_scan(P, 0, PROJ, io["ret_decay"][j], io, YF, Y)
                retention_scan(P, 1, PROJ, io["ret_decay"][j], io, YF, Y)
                if done("scan_%d" % layer):
                    return finish()
                project(P, Y, 4096, io["ret_w_out"][j], D, rows_dst(MIX), F32, all_tiles, "po")
            elif kind == 1:
                project(P, H, D, io["win_w_qkv"][j], 3072, rows_dst(PROJ[0]), BF16, all_tiles, "pq")
                attn_prep(P, PROJ[0], io, KT)
                attn_main(P, PROJ[0], io["win_sink"][j], io, KT, Y)
                project(P, Y[:, 0:2048], D, io["win_w_o"][j], D, rows_dst(MIX), F32, all_tiles, "po")
            elif kind == 2:
                project(P, H, D, io["c_fc"], 4096, rows_dst(PROJ[0]), BF16, all_tiles, "pf")
                fourier_pos(P, PROJ[0], io, Y)
                project(P, Y[:, 0:2048], D, io["fno_w_o"][j], D, rows_dst(MIX), F32, all_tiles, "po")
            if done("mix_%d" % layer):
                return finish()
            norm_phase(P, X, MOD, layer, 1, io["norm_gain"][layer, 1], H, resid=(MIX, 2), Xsrc=Xs)
            x_valid = True
        else:
            norm_phase(P, X, MOD, layer, 1, io["norm_gain"][layer, 1], H, Xsrc=Xs)
        if done("norm2_%d" % layer):
            return finish()
        project(P, H, D, io["router_w"][layer], NE, rows_dst(LOGITS), F32, all_tiles, "rt")
        if done("router_%d" % layer):
            return finish()
        with P.scope():
            R = {"aff": P.sb("R_aff", [128, NT, NE], F32), "idx": P.sb("R_idx", [128, NT, NE], I32), "d_aff": Dep(), "d_idx": Dep(),
                 "aff3": P.sb("R_aff3", [128, NT, NE, 3], BF16), "d_aff3": Dep(),
                 "sel": P.sb("R_sel", [128, NT, NE], F32), "d_sel": Dep()}
            routing_phase(P, LOGITS, None, R)
            if "IDXD" in dumps:
                P.dma("sp", IDXD[:, :], R["idx"][:].rearrange("p n e -> p (n e)"), reads=[R["d_idx"]])
                P.dma("sp", AFFD[:, :], R["aff"][:].rearrange("p n e -> p (n e)"), reads=[R["d_aff"]])
            if done("routing_%d" % layer):
                return finish()
            dispatch_phase(P, H, R, XE, XEC)
            if done("dispatch_%d" % layer):
                return finish()
            expert_phase(P, layer, io, XE, XEC, YE, YEC)
            if done("expert_%d" % layer):
                return finish()
            combine_norm_phase(P, X, MOD, layer, R, YE, YEC, 5, Xsrc=None if x_valid else io["x_in"])
            x_valid = True
        if done("layer_%d" % layer):
            return finish()
    norm_phase(P, X, None, 0, 0, io["final_gain"], None, final_out=out)
    return finish()


def make_in_maps(inputs, used=None):
    maps = []
    consts = make_consts(used)
    for b in range(2):
        m = {"x_in": np.ascontiguousarray(np.concatenate([inputs["ctx"][b], inputs["x"][b]], axis=0)),
             "cvec": np.ascontiguousarray(np.stack([inputs["c"][b], inputs["c_ctx"]], axis=0))}
        for k in WEIGHT_NAMES:
            if used is None or k in used:
                m[k] = np.ascontiguousarray(inputs[k])
        for k, v in consts.items():
            if used is not None and k in used:
                m[k] = v
        maps.append(m)
    return maps


def kernel(**inputs):
    inputs = {k: np.asarray(v) for k, v in inputs.items()}
    nc, used = build()
    res = run_bass_kernel_spmd(nc, make_in_maps(inputs, used), core_ids=[0, 1])
    return np.stack([res.results[b]["out"] for b in range(2)], axis=0).astype(np.float32)
```
